# Optimizing a Trainium2 kernel written in Bass

```python
import jax, jax.numpy as jnp
from jax import lax
import numpy as np

D_MODEL = 2048
BATCH = 2
SEQ = 4096
DEPTH = 1

EPS = 1e-6
NEG_INF = -1e30

RET_HEADS = 8
RET_QK_DIM = 128
RET_V_DIM = 256
RET_CHUNK = 128
RET_THETA = 10000.0
RET_QK_W = RET_HEADS * RET_QK_DIM
RET_V_W = RET_HEADS * RET_V_DIM

ATT_SEGMENTS = ((128, 1), (512, 4), (2048, 16))
ATT_HEADS_PER_GROUP = 8
ATT_HEAD_DIM = 128
ATT_ROT_DIM = ATT_HEAD_DIM // 4
ATT_THETA = 500000.0
ATT_HEADS = len(ATT_SEGMENTS) * ATT_HEADS_PER_GROUP
ATT_W = ATT_HEADS * ATT_HEAD_DIM
ATT_OUT_W = ATT_HEADS_PER_GROUP * ATT_HEAD_DIM

PEER_HEADS = 8
PEER_N_KEYS = 128
PEER_N_EXPERTS = PEER_N_KEYS * PEER_N_KEYS
PEER_QUERY_DIM = 256
PEER_TOPK = 16
PEER_TOKEN_BLOCK = 128

IN_SPLITS = (RET_QK_W, RET_QK_W, RET_V_W, RET_V_W, ATT_W, ATT_W, ATT_W, D_MODEL, D_MODEL)
IN_WIDTH = sum(IN_SPLITS)

kernel_name = 'hybrid_retention_dilated_attn_peer'


def rms_norm(x, g):
    xf = x.astype(jnp.float32)
    y = xf * lax.rsqrt(jnp.mean(xf * xf, axis=-1, keepdims=True) + EPS)
    return (y * g.astype(jnp.float32)).astype(x.dtype)


def rotary(x, rot_dim, theta):
    S = x.shape[1]
    half = rot_dim // 2
    freqs = theta ** (-jnp.arange(half, dtype=jnp.float32) / half)
    ang = jnp.arange(S, dtype=jnp.float32)[:, None] * freqs[None, :]
    cos = jnp.cos(ang)[None, :, None, :].astype(x.dtype)
    sin = jnp.sin(ang)[None, :, None, :].astype(x.dtype)
    x1 = x[..., :half]
    x2 = x[..., half:rot_dim]
    return jnp.concatenate([x1 * cos - x2 * sin, x2 * cos + x1 * sin, x[..., rot_dim:]], axis=-1)


def retention_direction(q, k, v, log_gamma, include_diag):
    B, S, H, dk = q.shape
    dv = v.shape[-1]
    C = RET_CHUNK
    nc = S // C
    qc = q.reshape(B, nc, C, H, dk).transpose(0, 1, 3, 2, 4)
    kc = k.reshape(B, nc, C, H, dk).transpose(0, 1, 3, 2, 4)
    vc = v.reshape(B, nc, C, H, dv).transpose(0, 1, 3, 2, 4)
    lg = log_gamma.astype(jnp.float32)
    pos = jnp.arange(C, dtype=jnp.float32)
    diff = pos[:, None] - pos[None, :]
    lower = (diff >= 0) if include_diag else (diff > 0)
    inner_decay = jnp.where(lower[None], jnp.exp(lg[:, None, None] * jnp.maximum(diff, 0.0)[None]), 0.0)
    q_decay = jnp.exp(lg[:, None] * (pos + 1.0)[None, :])
    k_decay = jnp.exp(lg[:, None] * (C - 1.0 - pos)[None, :])
    chunk_decay = jnp.exp(lg * C)
    scores = jnp.einsum('bnhid,bnhjd->bnhij', qc, kc) * inner_decay
    inner = jnp.einsum('bnhij,bnhje->bnhie', scores, vc)
    updates = jnp.einsum('bnhjd,hj,bnhje->bnhde', kc, k_decay, vc)

    def step(state, upd):
        return chunk_decay[None, :, None, None] * state + upd, state

    init = jnp.zeros((B, H, dk, dv), updates.dtype)
    _, prev = lax.scan(step, init, jnp.moveaxis(updates, 1, 0))
    prev = jnp.moveaxis(prev, 0, 1)
    cross = jnp.einsum('bnhid,bnhde->bnhie', qc, prev) * q_decay[None, None, :, :, None]
    out = inner + cross
    return out.transpose(0, 1, 3, 2, 4).reshape(B, S, H, dv)


def retention_branch(rq, rk, rv, rg, decay_fwd, decay_bwd, gn_g):
    B, S, _ = rq.shape
    q = rotary(rq.reshape(B, S, RET_HEADS, RET_QK_DIM), RET_QK_DIM, RET_THETA)
    k = rotary(rk.reshape(B, S, RET_HEADS, RET_QK_DIM), RET_QK_DIM, RET_THETA) * (RET_QK_DIM ** -0.5)
    v = rv.reshape(B, S, RET_HEADS, RET_V_DIM)
    lg_f = -jnp.exp(decay_fwd.astype(jnp.float32))
    lg_b = -jnp.exp(decay_bwd.astype(jnp.float32))
    fwd = retention_direction(q, k, v, lg_f, True)
    bwd = jnp.flip(retention_direction(jnp.flip(q, 1), jnp.flip(k, 1), jnp.flip(v, 1), lg_b, False), 1)
    o = (fwd + bwd).astype(jnp.float32)
    mu = jnp.mean(o, axis=-1, keepdims=True)
    var = jnp.mean(jnp.square(o - mu), axis=-1, keepdims=True)
    o = ((o - mu) * lax.rsqrt(var + EPS)).reshape(B, S, RET_V_W) * gn_g.astype(jnp.float32)
    return (jax.nn.silu(rg.astype(jnp.float32)) * o).astype(rg.dtype)


def dilated_window_attention(q, k, v, dilation, half):
    B, S, H, d = q.shape
    L = S // dilation
    N = B * dilation

    def by_residue(t):
        return t.reshape(B, L, dilation, H, d).transpose(0, 2, 1, 3, 4).reshape(N, L, H, d)

    qs, ks, vs = by_residue(q), by_residue(k), by_residue(v)
    blk = half
    nb = -(-L // blk)
    Lp = nb * blk
    qb = jnp.pad(qs, ((0, 0), (0, Lp - L), (0, 0), (0, 0))).reshape(N, nb, blk, H, d)
    kv_pad = ((0, 0), (blk, Lp - L + blk), (0, 0), (0, 0))
    kp = jnp.pad(ks, kv_pad).reshape(N, nb + 2, blk, H, d)
    vp = jnp.pad(vs, kv_pad).reshape(N, nb + 2, blk, H, d)
    kb = jnp.concatenate([kp[:, :-2], kp[:, 1:-1], kp[:, 2:]], axis=2)
    vb = jnp.concatenate([vp[:, :-2], vp[:, 1:-1], vp[:, 2:]], axis=2)
    s = jnp.einsum('nbihd,nbjhd->nbhij', qb, kb).astype(jnp.float32) * (d ** -0.5)
    qpos = jnp.arange(nb)[:, None] * blk + jnp.arange(blk)[None, :]
    kpos = jnp.arange(nb)[:, None] * blk - blk + jnp.arange(3 * blk)[None, :]
    rel = kpos[:, None, :] - qpos[:, :, None]
    valid = (jnp.abs(rel) <= half) & (kpos[:, None, :] >= 0) & (kpos[:, None, :] < L)
    s = jnp.where(valid[None, :, None], s, NEG_INF)
    m = jnp.max(s, axis=-1, keepdims=True)
    p = jnp.exp(s - m)
    l = jnp.sum(p, axis=-1, keepdims=True)
    o = jnp.einsum('nbhij,nbjhd->nbihd', p / l, vb)
    lse = (m + jnp.log(l))[..., 0].transpose(0, 1, 3, 2)
    o = o.reshape(N, Lp, H, d)[:, :L].reshape(B, dilation, L, H, d).transpose(0, 2, 1, 3, 4).reshape(B, S, H, d)
    lse = lse.reshape(N, Lp, H)[:, :L].reshape(B, dilation, L, H).transpose(0, 2, 1, 3).reshape(B, S, H)
    return o, lse


def dilated_attention_branch(aq, ak, av, q_norm_g, k_norm_g):
    B, S, _ = aq.shape
    shape = (B, S, ATT_HEADS, ATT_HEAD_DIM)
    q = rotary(rms_norm(aq.reshape(shape), q_norm_g), ATT_ROT_DIM, ATT_THETA)
    k = rotary(rms_norm(ak.reshape(shape), k_norm_g), ATT_ROT_DIM, ATT_THETA)
    v = av.reshape(shape)
    outs, lses = [], []
    for gi, (window, dilation) in enumerate(ATT_SEGMENTS):
        hs = slice(gi * ATT_HEADS_PER_GROUP, (gi + 1) * ATT_HEADS_PER_GROUP)
        o, lse = dilated_window_attention(q[:, :, hs], k[:, :, hs], v[:, :, hs], dilation, window // (2 * dilation))
        outs.append(o)
        lses.append(lse)
    w = jax.nn.softmax(jnp.stack(lses), axis=0)
    o = jnp.einsum('gbsh,gbshd->bshd', w, jnp.stack(outs))
    return o.reshape(B, S, ATT_OUT_W).astype(aq.dtype)


def peer_ffn(h, w_query, sub_keys, u, v):
    B, S, D = h.shape
    hq = PEER_QUERY_DIM // 2
    q = (h @ w_query).reshape(B, S, PEER_HEADS, PEER_QUERY_DIM).astype(jnp.float32)
    s1 = jnp.einsum('bshd,hkd->bshk', q[..., :hq], sub_keys[:, 0].astype(jnp.float32))
    s2 = jnp.einsum('bshd,hkd->bshk', q[..., hq:], sub_keys[:, 1].astype(jnp.float32))
    v1, i1 = lax.top_k(s1, PEER_TOPK)
    v2, i2 = lax.top_k(s2, PEER_TOPK)
    cand = (v1[..., :, None] + v2[..., None, :]).reshape(B, S, PEER_HEADS, PEER_TOPK * PEER_TOPK)
    best, pos = lax.top_k(cand, PEER_TOPK)
    e1 = jnp.take_along_axis(i1, pos // PEER_TOPK, axis=-1)
    e2 = jnp.take_along_axis(i2, pos % PEER_TOPK, axis=-1)
    experts = e1 * PEER_N_KEYS + e2
    gates = jax.nn.softmax(best, axis=-1)
    T = B * S
    nblk = T // PEER_TOKEN_BLOCK

    def block(args):
        hb, eb, gb = args
        a = jnp.einsum('td,thkd->thk', hb, u[eb])
        act = jax.nn.gelu(a.astype(jnp.float32), approximate=False) * gb
        return jnp.einsum('thk,thkd->td', act.astype(hb.dtype), v[eb])

    out = lax.map(block, (h.reshape(nblk, PEER_TOKEN_BLOCK, D),
                          experts.reshape(nblk, PEER_TOKEN_BLOCK, PEER_HEADS, PEER_TOPK),
                          gates.reshape(nblk, PEER_TOKEN_BLOCK, PEER_HEADS, PEER_TOPK)))
    return out.reshape(B, S, D)


def setup_inputs(seed: int = 0) -> dict:
    key = jax.random.key(seed)
    ks = jax.random.split(key, 16)
    f32 = jnp.float32

    def normal(k, shape, scale):
        return jax.random.normal(k, shape, f32) * scale

    heads = jnp.arange(RET_HEADS, dtype=f32)
    base = jnp.log(-jnp.log1p(-jnp.exp2(-5.0 - heads)))
    return {
        'x': normal(ks[0], (BATCH, SEQ, D_MODEL), 1.0),
        'mix_norm_g': 1.0 + normal(ks[1], (DEPTH, D_MODEL), 0.02),
        'w_in': normal(ks[2], (DEPTH, D_MODEL, IN_WIDTH), D_MODEL ** -0.5),
        'ret_decay_fwd': base + normal(ks[3], (DEPTH, RET_HEADS), 0.01),
        'ret_decay_bwd': base + normal(ks[4], (DEPTH, RET_HEADS), 0.01),
        'ret_gn_g': 1.0 + normal(ks[5], (DEPTH, RET_V_W), 0.02),
        'w_ret_out': normal(ks[6], (DEPTH, RET_V_W, D_MODEL), RET_V_W ** -0.5),
        'attn_q_norm_g': 1.0 + normal(ks[7], (DEPTH, ATT_HEAD_DIM), 0.02),
        'attn_k_norm_g': 1.0 + normal(ks[8], (DEPTH, ATT_HEAD_DIM), 0.02),
        'w_attn_out': normal(ks[9], (DEPTH, ATT_OUT_W, D_MODEL), ATT_OUT_W ** -0.5),
        'w_out': normal(ks[10], (DEPTH, D_MODEL, D_MODEL), D_MODEL ** -0.5),
        'ffn_norm_g': 1.0 + normal(ks[11], (DEPTH, D_MODEL), 0.02),
        'peer_w_query': normal(ks[12], (DEPTH, D_MODEL, PEER_HEADS * PEER_QUERY_DIM), D_MODEL ** -0.5),
        'peer_sub_keys': normal(ks[13], (DEPTH, PEER_HEADS, 2, PEER_N_KEYS, PEER_QUERY_DIM // 2), (PEER_QUERY_DIM // 2) ** -0.5),
        'peer_u': normal(ks[14], (DEPTH, PEER_N_EXPERTS, D_MODEL), D_MODEL ** -0.5),
        'peer_v': normal(ks[15], (DEPTH, PEER_N_EXPERTS, D_MODEL), PEER_TOPK ** -0.5),
    }


def reference(x, mix_norm_g, w_in, ret_decay_fwd, ret_decay_bwd, ret_gn_g, w_ret_out,
              attn_q_norm_g, attn_k_norm_g, w_attn_out, w_out, ffn_norm_g,
              peer_w_query, peer_sub_keys, peer_u, peer_v):
    split_at = np.cumsum(IN_SPLITS)[:-1].tolist()
    for l in range(DEPTH):
        h = rms_norm(x, mix_norm_g[l])
        proj = h @ w_in[l]
        rq, rk, rv, rg, aq, ak, av, gate_ret, gate_att = jnp.split(proj, split_at, axis=-1)
        ret = retention_branch(rq, rk, rv, rg, ret_decay_fwd[l], ret_decay_bwd[l], ret_gn_g[l]) @ w_ret_out[l]
        att = dilated_attention_branch(aq, ak, av, attn_q_norm_g[l], attn_k_norm_g[l]) @ w_attn_out[l]
        merged = jax.nn.sigmoid(gate_ret) * ret + jax.nn.sigmoid(gate_att) * att
        x = x + merged @ w_out[l]
        h2 = rms_norm(x, ffn_norm_g[l])
        x = x + peer_ffn(h2, peer_w_query[l], peer_sub_keys[l], peer_u[l], peer_v[l])
    return x
```

```python
import numpy as np
import ml_dtypes
import concourse.bass as bass
import concourse.mybir as mybir
from concourse.bass_utils import run_bass_kernel_spmd

F32 = mybir.dt.float32
BF16 = mybir.dt.bfloat16
ALU = mybir.AluOpType
AF = mybir.ActivationFunctionType

NCORES = 8
D = 2048
SEQ = 4096
NTOK = 8192
TOWN = 1024
EPS = 1e-6
SCL = float(128 ** -0.5)
DEBUG = False
SKIP = set()
STOP = 99


class Buf:
    __slots__ = ("name", "w", "r", "excl")

    def __init__(self, name="", excl=False):
        self.name = name
        self.excl = excl
        self.w = None
        self.r = {}


class Sched:
    K = 6

    def __init__(self, nc):
        self.nc = nc
        self.eng = {"pe": nc.tensor, "act": nc.scalar, "dve": nc.vector,
                    "pool": nc.gpsimd, "sp": nc.sync}
        self.sems = {}
        self.cnt = {}
        self.R = 10
        self.ep = 0
        self.semval = {}
        self.last_ev = {}
        for e in ("pe", "act", "dve", "pool"):
            for i in range(self.R):
                self.sems[("c", e, i)] = nc.alloc_semaphore("c_%s%d" % (e, i))
                self.semval[(e, i)] = 0
            self.cnt[e] = 0
        self.dma_uses = {}
        self.dma_n = {}
        for q in ("sp", "act", "pool"):
            self.dma_n[q] = 0
            for s in range(self.K):
                self.sems[("d", q, s)] = nc.alloc_semaphore("d_%s%d" % (q, s))
                self.dma_uses[(q, s)] = 0
        self.known = {e: {} for e in self.eng}
        self.ninst = 0
        self.nwaits = 0

    def _wait(self, e, evs):
        need = {}
        for key, val in evs:
            if e == "pe" and key[0] == "c" and key[1] == "pe":
                continue
            if self.known[e].get(key, 0) >= val:
                continue
            if need.get(key, 0) < val:
                need[key] = val
        for key, val in need.items():
            self.eng[e].wait_ge(self.sems[key], val)
            self.known[e][key] = val
            self.nwaits += 1

    @staticmethod
    def _deps(reads, writes):
        evs = []
        for b in reads:
            if b.w is not None:
                evs.append(b.w)
        for b in writes:
            if b.w is not None:
                evs.append(b.w)
            evs.extend(b.r.items())
        return evs

    @staticmethod
    def _mark(ev, reads, writes):
        key, val = ev
        for b in reads:
            if b.r.get(key, 0) < val:
                b.r[key] = val
        for b in writes:
            b.w = ev
            b.r = {}

    def op(self, e, reads, writes, emit):
        ex = [b for b in reads if b.excl]
        if ex:
            reads = [b for b in reads if not b.excl]
            writes = list(writes) + ex
        self._wait(e, self._deps(reads, writes))
        inst = emit(self.eng[e])
        self.cnt[e] += 1
        i = self.ep % self.R
        self.semval[(e, i)] += 1
        inst.then_inc(self.sems[("c", e, i)], 1)
        ev = (("c", e, i), self.semval[(e, i)])
        self.last_ev[e] = ev
        self._mark(ev, reads, writes)
        self.ninst += 1
        return inst

    def next_epoch(self):
        self.ep += 1

    def dma(self, q, out, in_, reads, writes, **kw):
        slot = self.dma_n[q] % self.K
        self.dma_n[q] += 1
        evs = self._deps(reads, writes)
        u = self.dma_uses[(q, slot)]
        if u > 0:
            evs.append((("d", q, slot), 16 * u))
        self._wait(q, evs)
        inst = self.eng[q].dma_start(out=out, in_=in_, **kw)
        inst.then_inc(self.sems[("d", q, slot)], 16)
        self.dma_uses[(q, slot)] = u + 1
        self._mark((("d", q, slot), 16 * (u + 1)), reads, writes)
        self.ninst += 1
        return inst

    def wait_bufs(self, e, reads, writes=()):
        self._wait(e, self._deps(list(reads), list(writes)))

    def drain(self, e):
        evs = []
        for en in ("pe", "act", "dve", "pool"):
            if en in self.last_ev:
                evs.append(self.last_ev[en])
        for (q, s), u in self.dma_uses.items():
            if u:
                evs.append((("d", q, s), 16 * u))
        self._wait(e, evs)


class Stack:
    def __init__(self, nc, sched=None):
        self.nc = nc
        self.live = []
        self.uid = 0
        self.sched = sched

    def mark(self):
        return len(self.live)

    def alloc(self, shape, dtype, name="t"):
        self.uid += 1
        g = self.nc.sbuf_tensor("%s_%d" % (name, self.uid), list(shape), dtype)
        h = g.__enter__()
        self.live.append(g)
        return h

    def release(self, mark):
        if self.sched is not None and len(self.live) > mark:
            for e in ("pe", "act", "dve", "pool", "sp"):
                self.sched.drain(e)
            self.sched.next_epoch()
        while len(self.live) > mark:
            g = self.live.pop()
            g.__exit__(None, None, None)


def build_program():
    nc = bass.Bass("TRN2", target_bir_lowering=False)
    S = Sched(nc)
    ST = Stack(nc, S)

    def finish():
        for e in ("sp", "pool", "act", "dve", "pe"):
            S.drain(e)
        print("program built: ninst", S.ninst, "nwaits", S.nwaits, S.cnt, S.dma_n, flush=True)
        return nc

    def din(name, shape, dt=F32):
        return nc.dram_tensor(name, list(shape), dt, kind="ExternalInput").ap()

    x_own = din("x_own", [TOWN, D])
    w_ret_d = din("w_ret", [D, 768])
    w_att_d = din("w_att", [D, 1152])
    if STOP > 1:
        w_gate_d = din("w_gate", [D, 4096])
        w_ret_out_d = din("w_ret_out", [D, D])
        w_attn_out_d = din("w_attn_out", [1024, D])
        w_out_d = din("w_out", [D, D])
        w_query_d = din("w_query", [D, D])
    if STOP > 2:
        NEXP = 1024 if "peersmall" in SKIP else 16384
        peer_u_d = din("peer_u", [NEXP, D])
        peer_v_d = din("peer_v", [NEXP, D])
    keysT_d = din("keysT", [128, 16, 128])
    g1_d = din("g1", [128, 16])
    g2_d = din("g2", [128, 16])
    small_d = din("small", [128, 8])
    ident_d = din("ident_bf", [128, 128], BF16)
    identf_d = din("ident_f", [128, 128])
    perm_d = din("perm", [128, 2, 128], BF16)
    rot_d = din("rot", [128, 4, SEQ])
    dmask_d = din("dmask", [128, 4, 128])
    amask_d = din("amask", [128, 384], BF16)
    pcol_d = din("pcol", [128, 4])
    prow_d = din("prow", [128, 2, 128])
    out_d = nc.dram_tensor("out", [TOWN, D], F32, kind="ExternalOutput").ap()
    if DEBUG:
        dbg_ab = nc.dram_tensor("dbg_ab", [384, NTOK], BF16, kind="ExternalOutput").ap()
        dbg_x1 = nc.dram_tensor("dbg_x1", [TOWN, D], F32, kind="ExternalOutput").ap()

    hT_in = nc.dram_tensor("hT_in", [D, TOWN], BF16)
    hT_all = nc.dram_tensor("hT_all", [NCORES * D, TOWN], BF16)
    AB_in = nc.dram_tensor("AB_in", [384, NTOK], BF16)
    AB_all = nc.dram_tensor("AB_all", [NCORES * 384, NTOK], BF16)
    b_hTin, b_hTall, b_ABin, b_ABall, b_out = Buf(), Buf(), Buf(), Buf(), Buf()

    pf = nc.alloc_psum_tensor("pf", [128, 6 * 512], F32)
    pb = [nc.alloc_psum_tensor("pb%d" % i, [128, 1024], BF16) for i in range(2)]
    b_pf = [Buf("pf%d" % i, True) for i in range(6)]
    b_pb = [Buf("pb0", True), Buf("pb1", True)]
    rr = {"f": 0, "b": 0}

    def fbank():
        i = rr["f"] % 6
        rr["f"] += 1
        return pf[:, i * 512:(i + 1) * 512], b_pf[i]

    def bbank():
        i = rr["b"] % 2
        rr["b"] += 1
        return pb[i], b_pb[i]

    def const(name, src, shape, dt, q="sp"):
        t = ST.alloc(shape, dt, name)
        b = Buf(name)
        S.dma(q, t.ap() if False else t[tuple(slice(None) for _ in shape)], src, [], [b])
        return t, b

    ident, b_ident = const("ident", ident_d, [128, 128], BF16)
    identf, b_identf = const("identf", identf_d, [128, 128], F32)
    perm, b_perm = const("perm", perm_d, [128, 2, 128], BF16)
    dmask, b_dmask = const("dmask", dmask_d, [128, 4, 128], F32)
    amask, b_amask = const("amask", amask_d, [128, 384], BF16)
    pcol, b_pcol = const("pcol", pcol_d, [128, 4], F32)
    prow, b_prow = const("prow", prow_d, [128, 2, 128], F32)
    g1, b_g1 = const("g1", g1_d, [128, 16], F32)
    g2, b_g2 = const("g2", g2_d, [128, 16], F32)
    small, b_small = const("small", small_d, [128, 8], F32)
    ones_bf = ST.alloc([128, 128], BF16, "ones")
    b_ones = Buf("ones")
    S.op("dve", [], [b_ones], lambda e: e.memset(ones_bf[:, :], 1.0))

    def rstd_from(ssum_ap, b_in, out_ap, b_out_, scale, eng_first="dve"):
        S.op("dve", [b_in], [b_out_], lambda e: e.tensor_scalar(
            out=out_ap, in0=ssum_ap, scalar1=scale, scalar2=EPS, op0=ALU.mult, op1=ALU.add))
        S.op("act", [b_out_], [b_out_], lambda e: e.activation(out=out_ap, in_=out_ap, func=AF.Sqrt))
        S.op("dve", [b_out_], [b_out_], lambda e: e.reciprocal(out=out_ap, in_=out_ap))

    def norm_tile_to_hT(xt, b_xt, gt, b_gt, dst_fn, b_dst, tmp):
        junk, b_junk, xn, b_xn, st, b_st = tmp
        S.op("act", [b_xt], [b_junk, b_st], lambda e: e.activation(
            out=junk[:, :], in_=xt, func=AF.Square, accum_out=st[:, 0:1]))
        rstd_from(st[:, 0:1], b_st, st[:, 1:2], b_st, 1.0 / D)
        S.op("act", [b_xt, b_st], [b_xn], lambda e: e.activation(
            out=xn[:, :], in_=xt, func=AF.Copy, scale=st[:, 1:2]))
        for half in range(2):
            bk, b_bk = bbank()
            for j in range(8):
                c = half * 8 + j
                S.op("pe", [b_xn, b_ident], [b_bk], lambda e, c=c, j=j: e.transpose(
                    out=bk[:, j * 128:(j + 1) * 128], in_=xn[:, c * 128:(c + 1) * 128], identity=ident[:, :]))
            S.op("dve", [b_bk, b_gt], [b_dst], lambda e, half=half: e.tensor_tensor(
                out=dst_fn(half * 8, half * 8 + 8),
                in0=bk[:, :].rearrange("p (c t) -> p c t", c=8),
                in1=gt[:, half * 8:half * 8 + 8].unsqueeze(2).to_broadcast([128, 8, 128]), op=ALU.mult))

    m0 = ST.mark()
    hTo = ST.alloc([128, 16, TOWN], BF16, "hTo")
    b_hTo = Buf("hTo")
    xs = [ST.alloc([128, D], F32, "xs") for _ in range(2)]
    b_xs = [Buf(), Buf()]
    junk = ST.alloc([128, D], F32, "junk"); b_junk = Buf()
    xn = ST.alloc([128, D], BF16, "xn"); b_xn = Buf()
    st = ST.alloc([128, 2], F32, "st"); b_st = Buf()
    for t in range(8):
        sl = t % 2
        S.dma("sp", xs[sl][:, :], x_own[t * 128:(t + 1) * 128, :], [], [b_xs[sl]])
        norm_tile_to_hT(xs[sl][:, :], b_xs[sl], g1, b_g1,
                        lambda c0, c1, t=t: hTo[:, c0:c1, t * 128:(t + 1) * 128], b_hTo,
                        (junk, b_junk, xn, b_xn, st, b_st))
    S.dma("sp", hT_in.ap().rearrange("(c p) t -> p c t", p=128), hTo[:, :, :], [b_hTo], [b_hTin])
    if STOP <= 0.1:
        return finish()
    ccsem = nc.alloc_semaphore("ccsem")
    S.wait_bufs("pool", [b_hTin], [b_hTall])
    nc.gpsimd.collective_compute("AllGather", ALU.bypass, replica_groups=[list(range(NCORES))],
                                 ins=[hT_in.ap().opt()], outs=[hT_all.ap().opt()]).then_inc(ccsem)
    for e in ("sp", "pool", "act"):
        S.eng[e].wait_ge(ccsem, 1)
    if STOP <= 0.2:
        return finish()
    ST.release(m0)

    m1 = ST.mark()
    dk = ST.alloc([128, 8], F32, "dk"); b_dk = Buf("dk")
    S.op("act", [b_small], [b_dk], lambda e: e.activation(out=dk[:, 0:2], in_=small[:, 0:2], func=AF.Exp))
    S.op("dve", [b_dk], [b_dk], lambda e: e.tensor_scalar(out=dk[:, 0:2], in0=dk[:, 0:2], scalar1=-1.0, scalar2=None, op0=ALU.mult))
    S.op("act", [b_dk, b_pcol], [b_dk], lambda e: e.activation(out=dk[:, 2:3], in_=pcol[:, 0:1], func=AF.Exp, scale=dk[:, 0:1]))
    S.op("act", [b_dk, b_pcol], [b_dk], lambda e: e.activation(out=dk[:, 3:4], in_=pcol[:, 1:2], func=AF.Exp, scale=dk[:, 1:2]))
    S.op("act", [b_dk, b_pcol], [b_dk], lambda e: e.activation(out=dk[:, 4:5], in_=pcol[:, 2:3], func=AF.Exp, scale=dk[:, 0:1]))
    S.op("act", [b_dk, b_pcol], [b_dk], lambda e: e.activation(out=dk[:, 5:6], in_=pcol[:, 2:3], func=AF.Exp, scale=dk[:, 1:2]))
    Dm = ST.alloc([128, 128], F32, "Dm"); b_Dm = Buf("Dm")
    Dt = ST.alloc([128, 128], F32, "Dt"); b_Dt = Buf("Dt")
    xi = ST.alloc([128, 2, 128], F32, "xi"); b_xi = Buf("xi")
    S.op("act", [b_dk, b_dmask], [b_Dm], lambda e: e.activation(out=Dm[:, :], in_=dmask[:, 0, :], func=AF.Exp, scale=dk[:, 0:1]))
    S.op("dve", [b_Dm, b_dmask], [b_Dm], lambda e: e.tensor_tensor(out=Dm[:, :], in0=Dm[:, :], in1=dmask[:, 1, :], op=ALU.mult))
    S.op("act", [b_dk, b_dmask], [b_Dt], lambda e: e.activation(out=Dt[:, :], in_=dmask[:, 2, :], func=AF.Exp, scale=dk[:, 1:2]))
    S.op("dve", [b_Dt, b_dmask], [b_Dt], lambda e: e.tensor_tensor(out=Dt[:, :], in0=Dt[:, :], in1=dmask[:, 3, :], op=ALU.mult))
    S.op("dve", [b_Dt, b_Dm], [b_Dm], lambda e: e.tensor_tensor(out=Dm[:, :], in0=Dm[:, :], in1=Dt[:, :], op=ALU.add))
    S.op("act", [b_dk, b_prow], [b_xi], lambda e: e.activation(out=xi[:, 0, :], in_=prow[:, 0, :], func=AF.Exp, scale=dk[:, 0:1]))
    S.op("act", [b_dk, b_prow], [b_xi], lambda e: e.activation(out=xi[:, 1, :], in_=prow[:, 1, :], func=AF.Exp, scale=dk[:, 1:2]))

    hTb = [ST.alloc([128, 16, 512], BF16, "hTb") for _ in range(2)]
    b_hTb = [Buf(), Buf()]
    rotb = [ST.alloc([128, 2, 512], F32, "rotb") for _ in range(2)]
    b_rotb = [Buf(), Buf()]
    blkn = {"n": 0}

    def load_block(b, kb, rot0):
        sl = blkn["n"] % 2
        blkn["n"] += 1
        r = b * 4 + kb // 2
        o = (kb % 2) * 512
        src = hT_all.ap()[r * D:(r + 1) * D, o:o + 512].rearrange("(c p) t -> p c t", p=128)
        S.dma("sp", hTb[sl][:, :, :], src, [b_hTall], [b_hTb[sl]])
        S.dma("sp", rotb[sl][:, :, :], rot_d[:, rot0:rot0 + 2, kb * 512:(kb + 1) * 512], [], [b_rotb[sl]])
        return hTb[sl], b_hTb[sl], rotb[sl], b_rotb[sl]

    def proj_fm(w_sb, b_w, c0, hb, b_hb):
        bk, b_bk = fbank()
        for k in range(16):
            S.op("pe", [b_w, b_hb], [b_bk], lambda e, k=k: e.matmul(
                bk, lhsT=w_sb[:, k, c0:c0 + 128], rhs=hb[:, k, :], start=(k == 0), stop=(k == 15)))
        return bk, b_bk

    tA = ST.alloc([128, 512], F32, "tA"); b_tA = Buf()
    tB = ST.alloc([128, 512], F32, "tB"); b_tB = Buf()
    tC = ST.alloc([128, 512], F32, "tC"); b_tC = Buf()
    qs = ST.alloc([128, 512], BF16, "qs"); b_qs = Buf()

    def rotary_store(src, b_src, scale, pidx, rt, b_rt, dst, b_dst, src_is_psum):
        S.op("act", [b_src], [b_qs], lambda e: e.activation(out=qs[:, :], in_=src, func=AF.Copy, scale=scale))
        bk, b_bk = fbank()
        S.op("pe", [b_perm, b_qs], [b_bk], lambda e: e.matmul(bk, lhsT=perm[:, pidx, :], rhs=qs[:, :], start=True, stop=True))
        S.op("dve", [b_src, b_rt], [b_tA], lambda e: e.tensor_tensor(out=tA[:, :], in0=src, in1=rt[:, 0, :], op=ALU.mult))
        S.op("dve", [b_bk, b_rt], [b_tB], lambda e: e.tensor_tensor(out=tB[:, :], in0=bk, in1=rt[:, 1, :], op=ALU.mult))
        S.op("dve", [b_tA, b_tB], [b_dst], lambda e: e.scalar_tensor_tensor(
            out=dst, in0=tA[:, :], scalar=scale, in1=tB[:, :], op0=ALU.mult, op1=ALU.add))

    for b in range(2):
        mR = ST.mark()
        w_ret = ST.alloc([128, 16, 768], BF16, "w_ret"); b_wret = Buf()
        S.dma("pool", w_ret[:, :, :], w_ret_d.rearrange("(c p) n -> p c n", p=128), [], [b_wret])
        qT = ST.alloc([128, SEQ], BF16, "qT"); b_qT = Buf()
        kT = ST.alloc([128, SEQ], BF16, "kT"); b_kT = Buf()
        vtm = ST.alloc([128, 32, 256], BF16, "vtm"); b_vtm = Buf()
        kf = ST.alloc([128, 32, 128], BF16, "kf"); b_kf = Buf()
        kbw = ST.alloc([128, 32, 128], BF16, "kbw"); b_kbw = Buf()
        rgT = ST.alloc([128, 2, SEQ], BF16, "rgT"); b_rgT = Buf()
        SBall = ST.alloc([128, 32, 256], BF16, "SBall"); b_SB = Buf()
        for kb in range(8):
            hb, b_hb, rt, b_rt = load_block(b, kb, 0)
            blk = slice(kb * 512, (kb + 1) * 512)
            if "rot" not in SKIP:
                bk, b_bk = proj_fm(w_ret, b_wret, 0, hb, b_hb)
                rotary_store(bk, b_bk, 1.0, 0, rt, b_rt, qT[:, blk], b_qT, True)
                bk, b_bk = proj_fm(w_ret, b_wret, 128, hb, b_hb)
                rotary_store(bk, b_bk, SCL, 0, rt, b_rt, kT[:, blk], b_kT, True)
            for t in range(4):
                if "v" in SKIP:
                    break
                c = kb * 4 + t
                bk, b_bk = fbank()
                for k in range(16):
                    S.op("pe", [b_wret, b_hb], [b_bk], lambda e, k=k, t=t: e.matmul(
                        bk[:, 0:256], lhsT=hb[:, k, t * 128:(t + 1) * 128], rhs=w_ret[:, k, 256:512],
                        start=(k == 0), stop=(k == 15)))
                S.op("act", [b_bk], [b_vtm], lambda e, c=c: e.activation(out=vtm[:, c, :], in_=bk[:, 0:256], func=AF.Copy))
            for h in range(2):
                if "rg" in SKIP:
                    break
                bk, b_bk = proj_fm(w_ret, b_wret, 512 + h * 128, hb, b_hb)
                S.op("act", [b_bk], [b_rgT], lambda e, h=h: e.activation(out=rgT[:, h, blk], in_=bk, func=AF.Silu))
            if "ktr" not in SKIP:
                bkb, b_bkb = bbank()
                for t in range(4):
                    S.op("pe", [b_kT, b_ident], [b_bkb], lambda e, t=t: e.transpose(
                        out=bkb[:, t * 128:(t + 1) * 128], in_=kT[:, kb * 512 + t * 128: kb * 512 + (t + 1) * 128], identity=ident[:, :]))
                S.op("act", [b_bkb, b_dk], [b_kf], lambda e: e.activation(
                    out=kf[:, kb * 4:kb * 4 + 4, :], in_=bkb[:, 0:512].rearrange("p (t d) -> p t d", t=4), func=AF.Copy, scale=dk[:, 2:3]))
                S.op("dve", [b_bkb, b_dk], [b_kbw], lambda e: e.tensor_scalar(
                    out=kbw[:, kb * 4:kb * 4 + 4, :], in0=bkb[:, 0:512].rearrange("p (t d) -> p t d", t=4),
                    scalar1=dk[:, 3:4], scalar2=None, op0=ALU.mult))
        if STOP <= 0.5:
            return finish()
        stt = ST.alloc([128, 256], F32, "stt"); b_stt = Buf()
        S.op("dve", [], [b_SB], lambda e: e.memset(SBall[:, 31, :], 0.0))
        for c in range(30, -1, -1):
            bk, b_bk = fbank()
            S.op("pe", [b_kbw, b_vtm], [b_bk], lambda e, c=c: e.matmul(
                bk[:, 0:256], lhsT=kbw[:, c + 1, :], rhs=vtm[:, c + 1, :], start=True, stop=True))
            if c == 30:
                S.op("dve", [b_bk], [b_stt], lambda e: e.tensor_copy(out=stt[:, :], in_=bk[:, 0:256]))
            else:
                S.op("dve", [b_stt, b_dk], [b_stt], lambda e: e.tensor_scalar(
                    out=stt[:, :], in0=stt[:, :], scalar1=dk[:, 5:6], scalar2=None, op0=ALU.mult))
                S.op("dve", [b_bk, b_stt], [b_stt], lambda e: e.tensor_tensor(
                    out=stt[:, :], in0=bk[:, 0:256], in1=stt[:, :], op=ALU.add))
            S.op("act", [b_stt], [b_SB], lambda e, c=c: e.activation(out=SBall[:, c, :], in_=stt[:, :], func=AF.Copy))
        sff = ST.alloc([128, 256], F32, "sff"); b_sff = Buf()
        sfb = [ST.alloc([128, 256], BF16, "sfb") for _ in range(2)]; b_sfb = [Buf(), Buf()]
        PT = [ST.alloc([128, 128], BF16, "PT") for _ in range(2)]; b_PT = [Buf(), Buf()]
        qfb = [ST.alloc([128, 2, 128], BF16, "qfb") for _ in range(2)]; b_qfb = [Buf(), Buf()]
        mv = ST.alloc([128, 8], F32, "mv"); b_mv = Buf()
        osb = ST.alloc([128, 256], F32, "osb"); b_osb = Buf()
        bst = ST.alloc([128, 8], F32, "bst"); b_bst = Buf()
        on = [ST.alloc([128, 256], BF16, "on") for _ in range(2)]; b_on = [Buf(), Buf()]
        ast = [ST.alloc([128, 2, 128], BF16, "ast") for _ in range(2)]; b_ast = [Buf(), Buf()]
        for c in range(32):
            s2 = c % 2
            tok = slice(c * 128, (c + 1) * 128)
            bk, b_bk = fbank()
            S.op("pe", [b_kT, b_qT], [b_bk], lambda e: e.matmul(bk[:, 0:128], lhsT=kT[:, tok], rhs=qT[:, tok], start=True, stop=True))
            S.op("dve", [b_bk, b_Dm], [b_PT[s2]], lambda e: e.tensor_tensor(out=PT[s2][:, :], in0=bk[:, 0:128], in1=Dm[:, :], op=ALU.mult))
            S.op("pool", [b_qT, b_xi], [b_qfb[s2]], lambda e: e.tensor_tensor(
                out=qfb[s2][:, :, :], in0=qT[:, tok].unsqueeze(1).to_broadcast([128, 2, 128]), in1=xi[:, :, :], op=ALU.mult))
            bo, b_bo = fbank()
            S.op("pe", [b_PT[s2], b_vtm], [b_bo], lambda e: e.matmul(bo[:, 0:256], lhsT=PT[s2][:, :], rhs=vtm[:, c, :], start=True, stop=False))
            if c > 0:
                S.op("pe", [b_qfb[s2], b_sfb[s2]], [b_bo], lambda e: e.matmul(bo[:, 0:256], lhsT=qfb[s2][:, 0, :], rhs=sfb[s2][:, :], start=False, stop=False))
            S.op("pe", [b_qfb[s2], b_SB], [b_bo], lambda e: e.matmul(bo[:, 0:256], lhsT=qfb[s2][:, 1, :], rhs=SBall[:, c, :], start=False, stop=True))
            S.op("act", [b_bo], [b_osb], lambda e: e.activation(out=osb[:, :], in_=bo[:, 0:256], func=AF.Copy))
            S.op("dve", [b_osb], [b_bst], lambda e: e.bn_stats(out=bst[:, 0:6], in_=osb[:, :]))
            S.op("dve", [b_bst], [b_mv], lambda e: e.bn_aggr(out=mv[:, 0:2], in_=bst[:, 0:6]))
            rstd_from(mv[:, 1:2], b_mv, mv[:, 2:3], b_mv, 1.0)
            S.op("dve", [b_osb, b_mv], [b_on[s2]], lambda e: e.tensor_scalar(
                out=on[s2][:, :], in0=osb[:, :], scalar1=mv[:, 0:1], scalar2=mv[:, 2:3], op0=ALU.subtract, op1=ALU.mult))
            bt_, b_bt = bbank()
            for h in range(2):
                S.op("pe", [b_on[s2], b_ident], [b_bt], lambda e, h=h: e.transpose(
                    out=bt_[:, h * 128:(h + 1) * 128], in_=on[s2][:, h * 128:(h + 1) * 128], identity=ident[:, :]))
            for h in range(2):
                S.op("dve", [b_bt, b_small], [b_ast[s2]], lambda e, h=h: e.tensor_scalar(
                    out=ast[s2][:, h, :], in0=bt_[:, h * 128:(h + 1) * 128], scalar1=small[:, 2 + h:3 + h], scalar2=None, op0=ALU.mult))
                S.op("pool", [b_rgT, b_ast[s2]], [b_ast[s2]], lambda e, h=h: e.tensor_tensor(
                    out=ast[s2][:, h, :], in0=ast[s2][:, h, :], in1=rgT[:, h, tok], op=ALU.mult))
            S.dma("sp", AB_in.ap()[0:256, b * SEQ + c * 128: b * SEQ + (c + 1) * 128].rearrange("(h p) t -> p h t", p=128),
                  ast[s2][:, :, :], [b_ast[s2]], [b_ABin])
            if c < 31:
                bu, b_bu = fbank()
                S.op("pe", [b_kf, b_vtm], [b_bu], lambda e: e.matmul(bu[:, 0:256], lhsT=kf[:, c, :], rhs=vtm[:, c, :], start=True, stop=True))
                if c == 0:
                    S.op("dve", [b_bu], [b_sff], lambda e: e.tensor_copy(out=sff[:, :], in_=bu[:, 0:256]))
                else:
                    S.op("dve", [b_sff, b_dk], [b_sff], lambda e: e.tensor_scalar(
                        out=sff[:, :], in0=sff[:, :], scalar1=dk[:, 4:5], scalar2=None, op0=ALU.mult))
                    S.op("dve", [b_bu, b_sff], [b_sff], lambda e: e.tensor_tensor(
                        out=sff[:, :], in0=bu[:, 0:256], in1=sff[:, :], op=ALU.add))
                n2 = (c + 1) % 2
                S.op("act", [b_sff], [b_sfb[n2]], lambda e: e.activation(out=sfb[n2][:, :], in_=sff[:, :], func=AF.Copy))
        if STOP <= 0.6:
            return finish()
        ST.release(mR)

        mA = ST.mark()
        w_att = ST.alloc([128, 16, 1152], BF16, "w_att"); b_watt = Buf()
        S.dma("pool", w_att[:, :, :], w_att_d.rearrange("(c p) n -> p c n", p=128), [], [b_watt])
        Nacc = ST.alloc([128, SEQ], F32, "Nacc"); b_N = Buf()
        Lacc = ST.alloc([128, SEQ], F32, "Lacc"); b_L = Buf()
        qTg = ST.alloc([128, SEQ], BF16, "qTg"); b_qTg = Buf()
        kTg = ST.alloc([128, SEQ], BF16, "kTg"); b_kTg = Buf()
        vTg = ST.alloc([128, SEQ], BF16, "vTg"); b_vTg = Buf()
        vres = ST.alloc([128, 32, 128], BF16, "vres"); b_vres = Buf()
        sq = ST.alloc([128, 512], BF16, "sq"); b_sq = Buf()
        PTa = [ST.alloc([128, 384], BF16, "PTa") for _ in range(4)]; b_PTa = [Buf() for _ in range(4)]
        for g in range(3):
            r = (1, 4, 16)[g]
            for kb in range(8):
                hb, b_hb, rt, b_rt = load_block(b, kb, 2)
                blk = slice(kb * 512, (kb + 1) * 512)
                for which in range(2):
                    dst, b_dst = (qTg, b_qTg) if which == 0 else (kTg, b_kTg)
                    bk, b_bk = proj_fm(w_att, b_watt, g * 384 + which * 128, hb, b_hb)
                    S.op("act", [b_bk], [b_sq], lambda e: e.activation(out=sq[:, :], in_=bk, func=AF.Square))
                    bs, b_bs = fbank()
                    S.op("pe", [b_ones, b_sq], [b_bs], lambda e: e.matmul(bs, lhsT=ones_bf[:, :], rhs=sq[:, :], start=True, stop=True))
                    rstd_from(bs, b_bs, tC[:, :], b_tC, 1.0 / 128)
                    S.op("dve", [b_bk, b_tC], [b_tC], lambda e: e.tensor_tensor(out=tC[:, :], in0=bk, in1=tC[:, :], op=ALU.mult))
                    S.op("dve", [b_small, b_tC], [b_tC], lambda e, which=which: e.tensor_scalar(
                        out=tC[:, :], in0=tC[:, :], scalar1=small[:, 4 + which:5 + which], scalar2=None, op0=ALU.mult))
                    rotary_store(tC[:, :], b_tC, 1.0, 1, rt, b_rt, dst[:, blk], b_dst, False)
                bk, b_bk = proj_fm(w_att, b_watt, g * 384 + 256, hb, b_hb)
                S.op("act", [b_bk], [b_vTg], lambda e: e.activation(out=vTg[:, blk], in_=bk, func=AF.Copy))
            if STOP <= 0.7:
                return finish()
            L = SEQ // r
            nt = L // 128
            for pr in range(r):
                def cols(n):
                    return slice(pr + r * 128 * n, pr + r * 128 * n + r * 127 + 1, r)
                for m0_ in range(0, nt, 8):
                    nn = min(8, nt - m0_)
                    bkb, b_bkb = bbank()
                    for j in range(nn):
                        S.op("pe", [b_vTg, b_ident], [b_bkb], lambda e, j=j: e.transpose(
                            out=bkb[:, j * 128:(j + 1) * 128], in_=vTg[:, cols(m0_ + j)], identity=ident[:, :]))
                    S.op("act", [b_bkb], [b_vres], lambda e: e.activation(
                        out=vres[:, m0_:m0_ + nn, :], in_=bkb[:, 0:nn * 128].rearrange("p (t d) -> p t d", t=nn), func=AF.Copy))
                for n0 in range(0, nt, 4):
                    ng = min(4, nt - n0)
                    info = []
                    for q in range(ng):
                        n = n0 + q
                        ms = [m for m in (n - 1, n, n + 1) if 0 <= m < nt]
                        bs, b_bs = fbank()
                        for m in ms:
                            o_ = (m - n + 1) * 128
                            S.op("pe", [b_kTg, b_qTg], [b_bs], lambda e, m=m, o_=o_: e.matmul(
                                bs[:, o_:o_ + 128], lhsT=kTg[:, cols(m)], rhs=qTg[:, cols(n)], start=True, stop=True))
                        c0 = (ms[0] - n + 1) * 128
                        c1 = (ms[-1] - n + 2) * 128
                        S.op("act", [b_bs], [b_PTa[q]], lambda e: e.activation(
                            out=PTa[q][:, c0:c1], in_=bs[:, c0:c1], func=AF.Exp, scale=SCL))
                        S.op("pool", [b_PTa[q], b_amask], [b_PTa[q]], lambda e: e.tensor_tensor(
                            out=PTa[q][:, c0:c1], in0=PTa[q][:, c0:c1], in1=amask[:, c0:c1], op=ALU.mult))
                        info.append((n, ms))
                    bO, b_bO = fbank()
                    bL, b_bL = fbank()
                    for q in range(ng):
                        n, ms = info[q]
                        for idx, m in enumerate(ms):
                            o_ = (m - n + 1) * 128
                            S.op("pe", [b_vres, b_PTa[q]], [b_bO], lambda e, m=m, o_=o_, idx=idx: e.matmul(
                                bO[:, q * 128:(q + 1) * 128], lhsT=vres[:, m, :], rhs=PTa[q][:, o_:o_ + 128],
                                start=(idx == 0), stop=(idx == len(ms) - 1)))
                        for idx, m in enumerate(ms):
                            o_ = (m - n + 1) * 128
                            S.op("pe", [b_ones, b_PTa[q]], [b_bL], lambda e, o_=o_, idx=idx: e.matmul(
                                bL[:, q * 128:(q + 1) * 128], lhsT=ones_bf[:, :], rhs=PTa[q][:, o_:o_ + 128],
                                start=(idx == 0), stop=(idx == len(ms) - 1)))
                    ts = slice(pr + r * 128 * n0, pr + r * 128 * n0 + r * (128 * ng - 1) + 1, r)
                    if g == 0:
                        S.op("act", [b_bO], [b_N], lambda e: e.activation(out=Nacc[:, ts], in_=bO[:, 0:ng * 128], func=AF.Copy))
                        S.op("dve", [b_bL], [b_L], lambda e: e.tensor_copy(out=Lacc[:, ts], in_=bL[:, 0:ng * 128]))
                    else:
                        S.op("dve", [b_bO, b_N], [b_N], lambda e: e.tensor_tensor(out=Nacc[:, ts], in0=bO[:, 0:ng * 128], in1=Nacc[:, ts], op=ALU.add))
                        S.op("dve", [b_bL, b_L], [b_L], lambda e: e.tensor_tensor(out=Lacc[:, ts], in0=bL[:, 0:ng * 128], in1=Lacc[:, ts], op=ALU.add))
        for i in range(4):
            sl = slice(i * 1024, (i + 1) * 1024)
            S.op("dve", [b_L], [b_L], lambda e: e.reciprocal(out=Lacc[:, sl], in_=Lacc[:, sl]))
            S.op("pool", [b_L, b_N], [b_qTg], lambda e: e.tensor_tensor(out=qTg[:, sl], in0=Nacc[:, sl], in1=Lacc[:, sl], op=ALU.mult))
        S.dma("sp", AB_in.ap()[256:384, b * SEQ:(b + 1) * SEQ], qTg[:, :], [b_qTg], [b_ABin])
        ST.release(mA)
    ST.release(m1)

    ccsem2 = nc.alloc_semaphore("ccsem2")
    S.wait_bufs("pool", [b_ABin], [b_ABall])
    nc.gpsimd.collective_compute("AllGather", ALU.bypass, replica_groups=[list(range(NCORES))],
                                 ins=[AB_in.ap().opt()], outs=[AB_all.ap().opt()]).then_inc(ccsem2)
    for e in ("sp", "pool", "act"):
        S.eng[e].wait_ge(ccsem2, 1)
    if DEBUG:
        S.dma("sp", dbg_ab, AB_in.ap(), [b_ABin], [Buf()])


    if STOP <= 1:
        return finish()
    pid = nc.sync.partition_id()
    m2 = ST.mark()
    mT = ST.alloc([128, 16, TOWN], BF16, "mT"); b_mT = Buf()
    mW = ST.mark()
    hTw = ST.alloc([128, 16, TOWN], BF16, "hTw"); b_hTw = Buf()
    S.dma("sp", hTw[:, :, :], hT_in.ap().rearrange("(c p) t -> p c t", p=128), [b_hTin], [b_hTw])
    AT = ST.alloc([128, 16, TOWN], BF16, "AT"); b_AT = Buf()
    BT = ST.alloc([128, 8, TOWN], BF16, "BT"); b_BT = Buf()
    for r in range(8):
        S.dma("sp", AT[:, 2 * r:2 * r + 2, :],
              AB_all.ap()[r * 384:r * 384 + 256, bass.ds(pid * TOWN, TOWN)].rearrange("(h p) t -> p h t", p=128),
              [b_ABall], [b_AT])
        S.dma("sp", BT[:, r, :], AB_all.ap()[r * 384 + 256:r * 384 + 384, bass.ds(pid * TOWN, TOWN)], [b_ABall], [b_BT])
    wq4 = [ST.alloc([128, 16, 512], BF16, "wq4") for _ in range(4)]
    b_wq4 = [Buf() for _ in range(4)]
    sg = [ST.alloc([128, 512], F32, "sg") for _ in range(2)]; b_sg = [Buf(), Buf()]
    mm_ = [ST.alloc([128, 512], F32, "mm") for _ in range(2)]; b_mm = [Buf(), Buf()]
    for qd in range(4):
        cs = slice(qd * 512, (qd + 1) * 512)
        S.dma("pool", wq4[0][:, :, :], w_ret_out_d[:, cs].rearrange("(c p) n -> p c n", p=128), [], [b_wq4[0]])
        S.dma("pool", wq4[1][:, 0:8, :], w_attn_out_d[:, cs].rearrange("(c p) n -> p c n", p=128), [], [b_wq4[1]])
        S.dma("pool", wq4[2][:, :, :], w_gate_d[:, qd * 512:(qd + 1) * 512].rearrange("(c p) n -> p c n", p=128), [], [b_wq4[2]])
        S.dma("pool", wq4[3][:, :, :], w_gate_d[:, 2048 + qd * 512:2048 + (qd + 1) * 512].rearrange("(c p) n -> p c n", p=128), [], [b_wq4[3]])
        for nt_ in range(4):
            ncol = slice(nt_ * 128, (nt_ + 1) * 128)
            for th in range(2):
                tsl = slice(th * 512, (th + 1) * 512)
                banks = []
                for wi, (nk, rhs_t, b_rhs) in enumerate(((16, AT, b_AT), (8, BT, b_BT), (16, hTw, b_hTw), (16, hTw, b_hTw))):
                    bk, b_bk = fbank()
                    for k in range(nk):
                        S.op("pe", [b_wq4[wi], b_rhs], [b_bk], lambda e, k=k, wi=wi, rhs_t=rhs_t, nk=nk, bk=bk: e.matmul(
                            bk, lhsT=wq4[wi][:, k, ncol], rhs=rhs_t[:, k, tsl], start=(k == 0), stop=(k == nk - 1)))
                    banks.append((bk, b_bk))
                for j in range(2):
                    S.op("act", [banks[2 + j][1]], [b_sg[j]], lambda e, j=j: e.activation(out=sg[j][:, :], in_=banks[2 + j][0], func=AF.Sigmoid))
                    S.op("dve", [banks[j][1], b_sg[j]], [b_mm[j]], lambda e, j=j: e.tensor_tensor(
                        out=mm_[j][:, :], in0=banks[j][0], in1=sg[j][:, :], op=ALU.mult))
                S.op("pool", [b_mm[0], b_mm[1]], [b_mT], lambda e: e.tensor_tensor(
                    out=mT[:, qd * 4 + nt_, tsl], in0=mm_[0][:, :], in1=mm_[1][:, :], op=ALU.add))
    ST.release(mW)
    mB = ST.mark()
    x1 = ST.alloc([128, 8, D], F32, "x1"); b_x1 = [Buf() for _ in range(8)]
    for t in range(8):
        S.dma("sp", x1[:, t, :], x_own[t * 128:(t + 1) * 128, :], [], [b_x1[t]])
    wo = [ST.alloc([128, 16, 512], BF16, "wo") for _ in range(2)]; b_wo = [Buf(), Buf()]
    for qd in range(4):
        sl = qd % 2
        S.dma("pool", wo[sl][:, :, :], w_out_d[:, qd * 512:(qd + 1) * 512].rearrange("(c p) n -> p c n", p=128), [], [b_wo[sl]])
        for t in range(8):
            bk, b_bk = fbank()
            for k in range(16):
                S.op("pe", [b_mT, b_wo[sl]], [b_bk], lambda e, k=k, t=t: e.matmul(
                    bk, lhsT=mT[:, k, t * 128:(t + 1) * 128], rhs=wo[sl][:, k, :], start=(k == 0), stop=(k == 15)))
            S.op("dve", [b_bk, b_x1[t]], [b_x1[t]], lambda e, t=t: e.tensor_tensor(
                out=x1[:, t, qd * 512:(qd + 1) * 512], in0=bk, in1=x1[:, t, qd * 512:(qd + 1) * 512], op=ALU.add))
    h2T = ST.alloc([128, 16, TOWN], BF16, "h2T"); b_h2T = Buf()
    junk = ST.alloc([128, D], F32, "junk2"); b_junk = Buf()
    xn = ST.alloc([128, D], BF16, "xn2"); b_xn = Buf()
    st = ST.alloc([128, 2], F32, "st2"); b_st = Buf()
    for t in range(8):
        S.dma("sp", out_d[t * 128:(t + 1) * 128, :], x1[:, t, :], [b_x1[t]], [b_out])
        if DEBUG:
            S.dma("sp", dbg_x1[t * 128:(t + 1) * 128, :], x1[:, t, :], [b_x1[t]], [Buf()])
        norm_tile_to_hT(x1[:, t, :], b_x1[t], g2, b_g2,
                        lambda c0, c1, t=t: h2T[:, c0:c1, t * 128:(t + 1) * 128], b_h2T,
                        (junk, b_junk, xn, b_xn, st, b_st))
    S.dma("sp", hT_in.ap().rearrange("(c p) t -> p c t", p=128), h2T[:, :, :], [b_h2T, b_hTw], [b_hTin])
    ST.release(mB)
    ST.release(m2)

    if STOP <= 2:
        return finish()
    m3 = ST.mark()
    for hp in range(2):
        mP = ST.mark()
        h2 = ST.alloc([128, 16, 512], BF16, "h2"); b_h2 = Buf()
        S.dma("sp", h2[:, :, :], hT_in.ap()[:, hp * 512:(hp + 1) * 512].rearrange("(c p) t -> p c t", p=128), [b_hTin], [b_h2])
        s1 = ST.alloc([128, 4, 8, 128], F32, "s1"); b_s1 = Buf()
        s2 = ST.alloc([128, 4, 8, 128], F32, "s2"); b_s2 = Buf()
        dg = ST.alloc([128, 4, 8, 128], BF16, "dg"); b_dg = Buf()
        acc = ST.alloc([128, 4, D], F32, "acc"); b_acc = [Buf() for _ in range(4)]
        mQ = ST.mark()
        keysT = ST.alloc([128, 16, 128], BF16, "keysT"); b_keys = Buf()
        S.dma("pool", keysT[:, :, :], keysT_d, [], [b_keys])
        qp = ST.alloc([128, 16, 512], BF16, "qp"); b_qp = Buf()
        wq = [ST.alloc([128, 16, 512], BF16, "wq") for _ in range(2)]; b_wq = [Buf(), Buf()]
        for qd in range(4):
            sl = qd % 2
            S.dma("pool", wq[sl][:, :, :], w_query_d[:, qd * 512:(qd + 1) * 512].rearrange("(c p) n -> p c n", p=128), [], [b_wq[sl]])
            for nt_ in range(4):
                bk, b_bk = fbank()
                for k in range(16):
                    S.op("pe", [b_wq[sl], b_h2], [b_bk], lambda e, k=k: e.matmul(
                        bk, lhsT=wq[sl][:, k, nt_ * 128:(nt_ + 1) * 128], rhs=h2[:, k, :],
                        start=(k == 0), stop=(k == 15)))
                S.op("act", [b_bk], [b_qp], lambda e: e.activation(out=qp[:, qd * 4 + nt_, :], in_=bk, func=AF.Copy))
        v16 = ST.alloc([128, 2, 16], F32, "v16"); b_v16 = Buf()
        wk = ST.alloc([128, 256], F32, "wk"); b_wk = Buf()
        cand = ST.alloc([128, 256], F32, "cand"); b_cand = Buf()
        best = ST.alloc([128, 16], F32, "best"); b_best = Buf()
        sc = ST.alloc([128, 4], F32, "sc"); b_sc = Buf()
        ej = ST.alloc([128, 16], F32, "ej"); b_ej = Buf()
        for tt in range(4):
            tsl = slice(tt * 128, (tt + 1) * 128)
            for h in range(8):
                for j, (sx, b_sx) in enumerate(((s1, b_s1), (s2, b_s2))):
                    bk, b_bk = fbank()
                    S.op("pe", [b_qp, b_keys], [b_bk], lambda e, j=j, h=h: e.matmul(
                        bk[:, 0:128], lhsT=qp[:, 2 * h + j, tsl], rhs=keysT[:, 2 * h + j, :], start=True, stop=True))
                    S.op("act", [b_bk], [b_sx], lambda e, sx=sx, h=h: e.activation(out=sx[:, tt, h, :], in_=bk[:, 0:128], func=AF.Copy))
                    S.op("dve", [b_sx], [b_v16], lambda e, sx=sx, j=j, h=h: e.max(out=v16[:, j, 0:8], in_=sx[:, tt, h, :]))
                    S.op("dve", [b_sx, b_v16], [b_wk], lambda e, sx=sx, j=j, h=h: e.match_replace(
                        out=wk[:, 0:128], in_to_replace=v16[:, j, 0:8], in_values=sx[:, tt, h, :], imm_value=-1e30))
                    S.op("dve", [b_wk], [b_v16], lambda e, j=j: e.max(out=v16[:, j, 8:16], in_=wk[:, 0:128]))
                S.op("dve", [b_v16], [b_cand], lambda e: e.tensor_tensor(
                    out=cand[:, :].rearrange("p (i j) -> p i j", i=16),
                    in0=v16[:, 0, :].unsqueeze(2).to_broadcast([128, 16, 16]),
                    in1=v16[:, 1, :].unsqueeze(1).to_broadcast([128, 16, 16]), op=ALU.add))
                S.op("dve", [b_cand], [b_best], lambda e: e.max(out=best[:, 0:8], in_=cand[:, :]))
                S.op("dve", [b_cand, b_best], [b_wk], lambda e: e.match_replace(
                    out=wk[:, :], in_to_replace=best[:, 0:8], in_values=cand[:, :], imm_value=-1e30))
                S.op("dve", [b_wk], [b_best], lambda e: e.max(out=best[:, 8:16], in_=wk[:, :]))
                S.op("dve", [b_best], [b_sc], lambda e: e.tensor_scalar(out=sc[:, 0:1], in0=best[:, 15:16], scalar1=-1.0, scalar2=None, op0=ALU.mult))
                S.op("act", [b_best, b_sc], [b_ej, b_sc], lambda e: e.activation(
                    out=ej[:, :], in_=best[:, :], func=AF.Exp, bias=sc[:, 0:1], scale=1.0, accum_out=sc[:, 1:2]))
                S.op("dve", [b_sc], [b_sc], lambda e: e.reciprocal(out=sc[:, 2:3], in_=sc[:, 1:2]))
                S.op("dve", [b_s2, b_sc], [b_s2], lambda e, h=h: e.tensor_scalar(
                    out=s2[:, tt, h, :], in0=s2[:, tt, h, :], scalar1=sc[:, 0:1], scalar2=None, op0=ALU.add))
                S.op("dve", [b_ident, b_sc], [b_dg], lambda e, h=h: e.tensor_scalar(
                    out=dg[:, tt, h, :], in0=ident[:, :], scalar1=sc[:, 2:3], scalar2=None, op0=ALU.mult))
        ST.release(mQ)
        ub = [ST.alloc([128, D], BF16, "ub") for _ in range(2)]; b_ub = [Buf(), Buf()]
        uT = [ST.alloc([128, 16, 128], BF16, "uT") for _ in range(2)]; b_uT = [Buf(), Buf()]
        vsb = [ST.alloc([128, 4, D], BF16, "vsb") for _ in range(2)]; b_vsb = [Buf(), Buf()]
        actT = [ST.alloc([128, 4, 512], BF16, "actT") for _ in range(2)]; b_actT = [Buf(), Buf()]
        Sx = [ST.alloc([128, 8, 128], F32, "Sx") for _ in range(2)]; b_Sx = [Buf(), Buf()]
        Px = [ST.alloc([128, 8, 128], F32, "Px") for _ in range(2)]; b_Px = [Buf(), Buf()]
        Gh = [ST.alloc([128, 8, 128], BF16, "Gh") for _ in range(2)]; b_Gh = [Buf(), Buf()]
        ge = [ST.alloc([128, 512], BF16, "ge") for _ in range(2)]; b_ge = [Buf(), Buf()]
        cnt = 0
        NSB = 0 if "nostream" in SKIP else (2 if "peersmall" in SKIP else 32)

        def load_v(sb_):
            S.dma("pool", vsb[sb_ % 2][:, :, :], peer_v_d[sb_ * 512:(sb_ + 1) * 512, :].rearrange("(c p) d -> p c d", p=128),
                  [], [b_vsb[sb_ % 2]])

        def load_u(e_):
            S.dma("pool", ub[e_ % 2][:, :], peer_u_d[e_ * 128:(e_ + 1) * 128, :], [], [b_ub[e_ % 2]])

        if NSB:
            load_u(0)
            load_v(0)
        for sb in range(NSB):
            vs = sb % 2
            if sb % 2 == 0:
                S.next_epoch()
            if sb + 1 < NSB:
                load_v(sb + 1)
            for ci in range(4):
                e1 = sb * 4 + ci
                u_ = e1 % 2
                if e1 + 1 < NSB * 4:
                    load_u(e1 + 1)
                for half in range(2):
                    bkb, b_bkb = bbank()
                    for j in range(8):
                        k = half * 8 + j
                        S.op("pe", [b_ub[u_], b_ident], [b_bkb], lambda e, j=j, k=k: e.transpose(
                            out=bkb[:, j * 128:(j + 1) * 128], in_=ub[u_][:, k * 128:(k + 1) * 128], identity=ident[:, :]))
                    S.op("act", [b_bkb], [b_uT[u_]], lambda e, half=half: e.activation(
                        out=uT[u_][:, half * 8:half * 8 + 8, :], in_=bkb[:, :].rearrange("p (c t) -> p c t", c=8), func=AF.Copy))
                ba = pf[:, 4 * 512:5 * 512]; b_ba = b_pf[4]
                for k in range(16):
                    S.op("pe", [b_uT[u_], b_h2], [b_ba], lambda e, k=k: e.matmul(
                        ba, lhsT=uT[u_][:, k, :], rhs=h2[:, k, :], start=(k == 0), stop=(k == 15)))
                S.op("act", [b_ba], [b_ge[u_]], lambda e: e.activation(out=ge[u_][:, :], in_=ba, func=AF.Gelu))
                bg = pf[:, 5 * 512:6 * 512]; b_bg = b_pf[5]
                for tt in range(4):
                    x_ = cnt % 2
                    cnt += 1
                    S.op("dve", [b_s1, b_s2], [b_Sx[x_]], lambda e, tt=tt: e.tensor_tensor(
                        out=Sx[x_][:, :, :], in0=s2[:, tt, :, :],
                        in1=s1[:, tt, :, e1:e1 + 1].to_broadcast([128, 8, 128]), op=ALU.add))
                    S.op("act", [b_Sx[x_]], [b_Px[x_]], lambda e: e.activation(out=Px[x_][:, :, :], in_=Sx[x_][:, :, :], func=AF.Exp))
                    S.op("dve", [b_Sx[x_], b_Px[x_]], [b_Gh[x_]], lambda e: e.scalar_tensor_tensor(
                        out=Gh[x_][:, :, :], in0=Sx[x_][:, :, :], scalar=0.0, in1=Px[x_][:, :, :], op0=ALU.is_ge, op1=ALU.mult))
                    for h in range(8):
                        S.op("pe", [b_Gh[x_], b_dg], [b_bg], lambda e, h=h, tt=tt: e.matmul(
                            bg[:, tt * 128:(tt + 1) * 128], lhsT=Gh[x_][:, h, :], rhs=dg[:, tt, h, :], start=(h == 0), stop=(h == 7)))
                S.op("dve", [b_bg, b_ge[u_]], [b_actT[vs]], lambda e: e.tensor_tensor(
                    out=actT[vs][:, ci, :], in0=bg, in1=ge[u_][:, :], op=ALU.mult))
            for tt in range(4):
                for db in range(4):
                    bo_ = pf[:, db * 512:(db + 1) * 512]
                    for ci in range(4):
                        S.op("pe", [b_actT[vs], b_vsb[vs]], [b_pf[db]], lambda e, ci=ci, db=db, tt=tt, bo_=bo_: e.matmul(
                            bo_, lhsT=actT[vs][:, ci, tt * 128:(tt + 1) * 128], rhs=vsb[vs][:, ci, db * 512:(db + 1) * 512],
                            start=(ci == 0), stop=(ci == 3)))
                for db in range(4):
                    bo_ = pf[:, db * 512:(db + 1) * 512]
                    dsl = slice(db * 512, (db + 1) * 512)
                    if sb == 0:
                        S.op("act", [b_pf[db]], [b_acc[tt]], lambda e, bo_=bo_, dsl=dsl, tt=tt: e.activation(
                            out=acc[:, tt, dsl], in_=bo_, func=AF.Copy))
                    else:
                        S.op("dve", [b_pf[db], b_acc[tt]], [b_acc[tt]], lambda e, bo_=bo_, dsl=dsl, tt=tt: e.tensor_tensor(
                            out=acc[:, tt, dsl], in0=bo_, in1=acc[:, tt, dsl], op=ALU.add))
        xr = [ST.alloc([128, D], F32, "xr") for _ in range(2)]; b_xr = [Buf(), Buf()]
        for tt in range(4):
            tglob = hp * 4 + tt
            rs = slice(tglob * 128, (tglob + 1) * 128)
            xs_ = tt % 2
            S.dma("sp", xr[xs_][:, :], out_d[rs, :], [b_out], [b_xr[xs_]])
            if "nostream" not in SKIP:
                S.op("dve", [b_xr[xs_], b_acc[tt]], [b_xr[xs_]], lambda e, tt=tt: e.tensor_tensor(
                    out=xr[xs_][:, :], in0=xr[xs_][:, :], in1=acc[:, tt, :], op=ALU.add))
            S.dma("sp", out_d[rs, :], xr[xs_][:, :], [b_xr[xs_]], [b_out])
        ST.release(mP)
    ST.release(m3)
    for e in ("sp", "pool", "act", "dve", "pe"):
        S.drain(e)
    print("program built: ninst", S.ninst, "nwaits", S.nwaits, S.cnt, S.dma_n, flush=True)
    return nc


_PROG = {}


def _constants():
    f32 = np.float32
    c = {}
    c["ident_bf"] = np.eye(128, dtype=f32).astype(ml_dtypes.bfloat16)
    c["ident_f"] = np.eye(128, dtype=f32)
    Pr = np.zeros((128, 128), f32)
    for m in range(128):
        Pr[(m + 64) % 128, m] = 1.0
    Pa = np.zeros((128, 128), f32)
    for m in range(16):
        Pa[m + 16, m] = 1.0
        Pa[m, m + 16] = 1.0
    c["perm"] = np.stack([Pr, Pa], axis=1).astype(ml_dtypes.bfloat16)
    pos = np.arange(SEQ, dtype=f32)
    rot = np.zeros((128, 4, SEQ), f32)
    fr = (np.float32(10000.0) ** (-np.arange(64, dtype=f32) / np.float32(64))).astype(f32)
    ang = (pos[None, :] * fr[:, None]).astype(f32)
    rot[0:64, 0] = np.cos(ang); rot[64:128, 0] = np.cos(ang)
    rot[0:64, 1] = -np.sin(ang); rot[64:128, 1] = np.sin(ang)
    fa = (np.float32(500000.0) ** (-np.arange(16, dtype=f32) / np.float32(16))).astype(f32)
    anga = (pos[None, :] * fa[:, None]).astype(f32)
    rot[:, 2] = 1.0
    rot[0:16, 2] = np.cos(anga); rot[16:32, 2] = np.cos(anga)
    rot[0:16, 3] = -np.sin(anga); rot[16:32, 3] = np.sin(anga)
    c["rot"] = rot
    j = np.arange(128, dtype=f32)[:, None]
    i = np.arange(128, dtype=f32)[None, :]
    dm = np.zeros((128, 4, 128), f32)
    dm[:, 0] = np.maximum(i - j, 0); dm[:, 1] = (i >= j)
    dm[:, 2] = np.maximum(j - i, 0); dm[:, 3] = (j > i)
    c["dmask"] = dm
    am = np.zeros((128, 384), f32)
    for d_ in (-1, 0, 1):
        am[:, (d_ + 1) * 128:(d_ + 2) * 128] = (np.abs(128 * d_ + j - i) <= 64)
    c["amask"] = am.astype(ml_dtypes.bfloat16)
    pc = np.zeros((128, 4), f32)
    pc[:, 0] = 127 - np.arange(128); pc[:, 1] = np.arange(128); pc[:, 2] = 128.0
    c["pcol"] = pc
    pr = np.zeros((128, 2, 128), f32)
    pr[:, 0, :] = np.arange(128)[None, :] + 1.0
    pr[:, 1, :] = 128.0 - np.arange(128)[None, :]
    c["prow"] = pr
    return c


def make_in_maps(x, mix_norm_g, w_in, ret_decay_fwd, ret_decay_bwd, ret_gn_g, w_ret_out,
                 attn_q_norm_g, attn_k_norm_g, w_attn_out, w_out, ffn_norm_g,
                 peer_w_query, peer_sub_keys, peer_u, peer_v):
    f32 = np.float32
    cst = _constants()
    xf = np.asarray(x, f32).reshape(NTOK, D)
    w_in0 = np.asarray(w_in, f32)[0]
    shared = dict(cst)
    shared["w_gate"] = np.ascontiguousarray(w_in0[:, 15360:19456])
    shared["w_ret_out"] = np.ascontiguousarray(np.asarray(w_ret_out, f32)[0])
    shared["w_attn_out"] = np.ascontiguousarray(np.asarray(w_attn_out, f32)[0])
    shared["w_out"] = np.ascontiguousarray(np.asarray(w_out, f32)[0])
    shared["w_query"] = np.ascontiguousarray(np.asarray(peer_w_query, f32)[0])
    shared["peer_u"] = np.ascontiguousarray(np.asarray(peer_u, f32)[0])
    shared["peer_v"] = np.ascontiguousarray(np.asarray(peer_v, f32)[0])
    sk = np.asarray(peer_sub_keys, f32)[0]
    shared["keysT"] = np.ascontiguousarray(sk.reshape(16, 128, 128).transpose(2, 0, 1))
    shared["g1"] = np.ascontiguousarray(np.asarray(mix_norm_g, f32)[0].reshape(16, 128).T)
    shared["g2"] = np.ascontiguousarray(np.asarray(ffn_norm_g, f32)[0].reshape(16, 128).T)
    gn = np.asarray(ret_gn_g, f32)[0]
    qg = np.asarray(attn_q_norm_g, f32)[0]
    kg = np.asarray(attn_k_norm_g, f32)[0]
    df = np.asarray(ret_decay_fwd, f32)[0]
    db = np.asarray(ret_decay_bwd, f32)[0]
    maps = []
    for c in range(NCORES):
        m = dict(shared)
        m["x_own"] = np.ascontiguousarray(xf[c * TOWN:(c + 1) * TOWN])
        m["w_ret"] = np.ascontiguousarray(np.concatenate([
            w_in0[:, c * 128:(c + 1) * 128], w_in0[:, 1024 + c * 128:1024 + (c + 1) * 128],
            w_in0[:, 2048 + c * 256:2048 + (c + 1) * 256], w_in0[:, 4096 + c * 256:4096 + (c + 1) * 256]], axis=1))
        cols = []
        for g in range(3):
            hh = g * 8 + c
            for base in (6144, 9216, 12288):
                cols.append(w_in0[:, base + hh * 128: base + (hh + 1) * 128])
        m["w_att"] = np.ascontiguousarray(np.concatenate(cols, axis=1))
        sm = np.zeros((128, 8), f32)
        sm[:, 0] = df[c]; sm[:, 1] = db[c]
        sm[:, 2] = gn[c * 256:c * 256 + 128]; sm[:, 3] = gn[c * 256 + 128:c * 256 + 256]
        sm[:, 4] = qg; sm[:, 5] = kg
        m["small"] = sm
        maps.append(m)
    return maps


def kernel(**inputs):
    if "nc" not in _PROG:
        _PROG["nc"] = build_program()
    nc = _PROG["nc"]
    in_maps = make_in_maps(**inputs)
    res = run_bass_kernel_spmd(nc, in_maps, core_ids=list(range(NCORES)))
    _PROG["last"] = res
    out = np.concatenate([res.results[c]["out"] for c in range(NCORES)], axis=0)
    return out.reshape(2, SEQ, D).astype(np.float32)
```
